# Optimizing a Trainium2 kernel written in Bass

```python
import jax
import jax.numpy as jnp
from jax import lax
import numpy as np

D_MODEL = 1024
BATCH = 4
SEQ = 8192
DEPTH = 2

HEAD_DIM = 64
ROT_DIM = HEAD_DIM // 4
ROPE_THETA = 500000.0
NORM_EPS = 1e-6
Q_BLOCK = 128
D_FF = 2816

SB_HEADS = 4
NSA_HEADS = 8
NSA_GROUPS = 2
NSA_HPG = NSA_HEADS // NSA_GROUPS
NSA_CMP_LEN = 32
NSA_CMP_STRIDE = 16
NSA_CMP_HIDDEN = 2 * HEAD_DIM
NSA_SEL_LEN = 64
NSA_SEL_N = 16
NSA_WINDOW = 512
NSA_FORCED_SCORE = 1.0e4
DSA_HEADS = 4
DSA_IDX_HEADS = 8
DSA_IDX_DIM = 32
DSA_IDX_ROT = DSA_IDX_DIM // 4
DSA_TOPK = 256

SB_W = SB_HEADS * HEAD_DIM
NSA_QW = NSA_HEADS * HEAD_DIM
NSA_KVW = NSA_GROUPS * HEAD_DIM
DSA_QW = DSA_HEADS * HEAD_DIM
IN_WIDTHS = (SB_W, SB_W, SB_W,
             NSA_QW, NSA_KVW, NSA_KVW, NSA_KVW, NSA_KVW, NSA_KVW, NSA_KVW, 3 * NSA_HEADS,
             DSA_QW, HEAD_DIM, HEAD_DIM, DSA_IDX_HEADS * DSA_IDX_DIM, DSA_IDX_DIM, DSA_IDX_HEADS)
D_IN = sum(IN_WIDTHS)

kernel_name = "hybrid_sb_nsa_dsa_macaron"


def rms_norm(x, g):
    xf = x.astype(jnp.float32)
    y = xf * lax.rsqrt(jnp.mean(xf * xf, axis=-1, keepdims=True) + NORM_EPS)
    return (y * g.astype(jnp.float32)).astype(x.dtype)


def rope_tables(pos, rot_dim):
    inv = ROPE_THETA ** (-jnp.arange(0, rot_dim, 2, dtype=jnp.float32) / rot_dim)
    ang = pos.astype(jnp.float32)[:, None] * inv[None, :]
    return jnp.cos(ang), jnp.sin(ang)


def apply_rope(x, cos, sin):
    half = cos.shape[-1]
    xf = x[..., :2 * half].astype(jnp.float32)
    x1, x2 = xf[..., :half], xf[..., half:]
    c, s = cos[:, None, :], sin[:, None, :]
    rot = jnp.concatenate([x1 * c - x2 * s, x2 * c + x1 * s], axis=-1).astype(x.dtype)
    return jnp.concatenate([rot, x[..., 2 * half:]], axis=-1)


def masked_softmax(s, mask):
    s = jnp.where(mask, s.astype(jnp.float32), -jnp.inf)
    m = jnp.max(s, axis=-1, keepdims=True)
    m = jnp.where(jnp.isfinite(m), m, 0.0)
    p = jnp.exp(s - m)
    den = jnp.sum(p, axis=-1, keepdims=True)
    return p / jnp.where(den > 0, den, 1.0)


def unblock(y):
    n, b, t = y.shape[:3]
    return jnp.moveaxis(y, 0, 1).reshape((b, n * t) + y.shape[3:])


def swiglu_ffn(x, g, w_gu, w_down):
    h = rms_norm(x, g)
    gate, up = jnp.split(h @ w_gu, 2, axis=-1)
    return (jax.nn.silu(gate) * up) @ w_down


def stick_breaking_attention(q, k, v):
    B, S, H, d = q.shape
    scale = d ** -0.5
    kh = k.transpose(0, 2, 1, 3)
    vh = v.transpose(0, 2, 1, 3)
    s_pos = jnp.arange(S)

    def block(i):
        q0 = i * Q_BLOCK
        qb = lax.dynamic_slice_in_dim(q, q0, Q_BLOCK, axis=1)
        z = jnp.einsum('bthd,bhsd->bhts', qb, kh).astype(jnp.float32) * scale
        t_pos = q0 + jnp.arange(Q_BLOCK)
        past = s_pos[None, :] < t_pos[:, None]
        log_1m = jnp.where(past, jax.nn.log_sigmoid(-z), 0.0)
        tail = lax.cumsum(log_1m, axis=3, reverse=True) - log_1m
        w = jnp.where(past, jnp.exp(jax.nn.log_sigmoid(z) + tail), 0.0)
        return jnp.einsum('bhts,bhsd->bthd', w.astype(v.dtype), vh)

    return unblock(lax.map(block, jnp.arange(S // Q_BLOCK)))


def nsa_compress(x, pe, w1, w2):
    B, S, G, d = x.shape
    nchunk = S // NSA_CMP_STRIDE
    r = NSA_CMP_LEN // NSA_CMP_STRIDE
    nc = nchunk - r + 1
    chunks = x.reshape(B, nchunk, NSA_CMP_STRIDE, G, d)
    blocks = jnp.concatenate([chunks[:, m:m + nc] for m in range(r)], axis=2)
    blocks = blocks + pe[None, None, :, None, :]
    flat = blocks.transpose(0, 1, 3, 2, 4).reshape(B, nc, G, NSA_CMP_LEN * d)
    return jax.nn.silu(flat @ w1) @ w2


def nsa_attention(q, k_cmp, v_cmp, k_sel, v_sel, k_win, v_win, gate_logits,
                  q_norm, k_norm, pe, w1, w2, cos, sin):
    B, S, _, d = q.shape
    G, hpg, T = NSA_GROUPS, NSA_HPG, Q_BLOCK
    scale = d ** -0.5
    dt = v_sel.dtype
    qg = apply_rope(rms_norm(q, q_norm), cos, sin).reshape(B, S, G, hpg, d)
    k_sel = apply_rope(rms_norm(k_sel, k_norm[1]), cos, sin)
    k_win = apply_rope(rms_norm(k_win, k_norm[2]), cos, sin)
    gates = jax.nn.sigmoid(gate_logits).reshape(B, S, G, hpg, 3)

    kc = rms_norm(nsa_compress(k_cmp, pe[0], w1[0], w2[0]), k_norm[0])
    nc = kc.shape[1]
    cmp_start = jnp.arange(nc) * NSA_CMP_STRIDE
    cmp_end = cmp_start + NSA_CMP_LEN - 1
    cos_c, sin_c = rope_tables(cmp_end, ROT_DIM)
    kc = apply_rope(kc, cos_c, sin_c).transpose(0, 2, 1, 3)
    vc = nsa_compress(v_cmp, pe[1], w1[1], w2[1]).transpose(0, 2, 1, 3)

    ns = S // NSA_SEL_LEN
    n_sel = min(NSA_SEL_N, ns)
    ks_blocks = k_sel.reshape(B, ns, NSA_SEL_LEN, G, d).transpose(0, 3, 1, 2, 4)
    vs_blocks = v_sel.reshape(B, ns, NSA_SEL_LEN, G, d).transpose(0, 3, 1, 2, 4)
    sel_start = jnp.arange(ns) * NSA_SEL_LEN
    overlap = ((cmp_start[:, None] < sel_start[None, :] + NSA_SEL_LEN)
               & (cmp_end[:, None] >= sel_start[None, :])).astype(jnp.float32)
    gather_blocks = jax.vmap(jax.vmap(lambda src, ix: src[ix]))

    pad = ((0, 0), (NSA_WINDOW, 0), (0, 0), (0, 0))
    kw_pad = jnp.pad(k_win, pad)
    vw_pad = jnp.pad(v_win, pad)

    def block(i):
        q0 = i * T
        t_pos = q0 + jnp.arange(T)
        qb = lax.dynamic_slice_in_dim(qg, q0, T, axis=1)

        sc = jnp.einsum('btghd,bgcd->bghtc', qb, kc) * scale
        pc = masked_softmax(sc, cmp_end[None, :] <= t_pos[:, None])
        o_cmp = jnp.einsum('bghtc,bgcd->btghd', pc.astype(dt), vc)

        imp = jnp.einsum('bghtc,cn->bgtn', pc, overlap)
        j = jnp.arange(ns)[None, :]
        cur = (t_pos // NSA_SEL_LEN)[:, None]
        visible = j * NSA_SEL_LEN <= t_pos[:, None]
        forced = (j == 0) | (j == cur) | (j == cur - 1)
        score = jnp.where(visible, jnp.where(forced, NSA_FORCED_SCORE, imp), -jnp.inf)
        _, sel = lax.top_k(score, n_sel)
        k_g = gather_blocks(ks_blocks, sel)
        v_g = gather_blocks(vs_blocks, sel).reshape(B, G, T, n_sel * NSA_SEL_LEN, d)
        tok = sel[..., None] * NSA_SEL_LEN + jnp.arange(NSA_SEL_LEN)
        smask = (tok <= t_pos[:, None, None]).reshape(B, G, 1, T, n_sel * NSA_SEL_LEN)
        ss = jnp.einsum('btghd,bgtnld->bghtnl', qb, k_g) * scale
        ps = masked_softmax(ss.reshape(B, G, hpg, T, n_sel * NSA_SEL_LEN), smask)
        o_sel = jnp.einsum('bghtm,bgtmd->btghd', ps.astype(dt), v_g)

        kwb = lax.dynamic_slice_in_dim(kw_pad, q0, T + NSA_WINDOW, axis=1)
        vwb = lax.dynamic_slice_in_dim(vw_pad, q0, T + NSA_WINDOW, axis=1)
        s_abs = q0 - NSA_WINDOW + jnp.arange(T + NSA_WINDOW)
        wmask = ((s_abs[None, :] <= t_pos[:, None]) & (s_abs[None, :] > t_pos[:, None] - NSA_WINDOW)
                 & (s_abs[None, :] >= 0))
        sw = jnp.einsum('btghd,bsgd->bghts', qb, kwb) * scale
        pw = masked_softmax(sw, wmask)
        o_win = jnp.einsum('bghts,bsgd->btghd', pw.astype(dt), vwb)

        g = lax.dynamic_slice_in_dim(gates, q0, T, axis=1)
        return g[..., 0:1] * o_cmp + g[..., 1:2] * o_sel + g[..., 2:3] * o_win

    out = unblock(lax.map(block, jnp.arange(S // T)))
    return out.reshape(B, S, NSA_HEADS * d)


def dsa_attention(q, k, v, iq, ik, iw, q_norm, k_norm, cos, sin, cos_i, sin_i):
    B, S, _, d = q.shape
    T = Q_BLOCK
    scale = d ** -0.5
    dt = v.dtype
    top_k = min(DSA_TOPK, S // 4)
    q = apply_rope(rms_norm(q, q_norm), cos, sin)
    k = apply_rope(rms_norm(k[:, :, None, :], k_norm), cos, sin)[:, :, 0, :]
    iq = apply_rope(iq, cos_i, sin_i)
    ik = apply_rope(ik[:, :, None, :], cos_i, sin_i)[:, :, 0, :]
    iw = iw.astype(jnp.float32) * (DSA_IDX_HEADS ** -0.5) * (DSA_IDX_DIM ** -0.5)
    s_pos = jnp.arange(S)
    gather_rows = jax.vmap(lambda src, ix: src[ix])

    def block(i):
        q0 = i * T
        t_pos = q0 + jnp.arange(T)
        qi = lax.dynamic_slice_in_dim(iq, q0, T, axis=1)
        wi = lax.dynamic_slice_in_dim(iw, q0, T, axis=1)
        logits = jax.nn.relu(jnp.einsum('bthd,bsd->bths', qi, ik).astype(jnp.float32))
        score = jnp.einsum('bths,bth->bts', logits, wi)
        score = jnp.where(s_pos[None, None, :] <= t_pos[None, :, None], score, -jnp.inf)
        _, idx = lax.top_k(score, top_k)
        kg = gather_rows(k, idx)
        vg = gather_rows(v, idx)
        qb = lax.dynamic_slice_in_dim(q, q0, T, axis=1)
        sc = jnp.einsum('bthd,btkd->bhtk', qb, kg) * scale
        p = masked_softmax(sc, (idx <= t_pos[None, :, None])[:, None])
        return jnp.einsum('bhtk,btkd->bthd', p.astype(dt), vg)

    out = unblock(lax.map(block, jnp.arange(S // T)))
    return out.reshape(B, S, DSA_HEADS * d)


def hybrid_mixer(h, w_in, w_gate, nsa_q_norm, nsa_k_norm, nsa_cmp_pe, nsa_cmp_w1, nsa_cmp_w2,
                 dsa_q_norm, dsa_k_norm, w_br_a, w_br_b, w_br_c, w_out, cos, sin, cos_i, sin_i):
    B, S, _ = h.shape
    offs = np.cumsum(IN_WIDTHS)[:-1].tolist()
    (sb_q, sb_k, sb_v, nq, nkc, nvc, nks, nvs, nkw, nvw, ngate,
     dq, dk, dv, iq, ik, iw) = jnp.split(h @ w_in, offs, axis=-1)

    def heads(t, n, d):
        return t.reshape(B, S, n, d)

    o_a = stick_breaking_attention(heads(sb_q, SB_HEADS, HEAD_DIM), heads(sb_k, SB_HEADS, HEAD_DIM),
                                   heads(sb_v, SB_HEADS, HEAD_DIM)).reshape(B, S, SB_W)
    kv = lambda t: heads(t, NSA_GROUPS, HEAD_DIM)
    o_b = nsa_attention(heads(nq, NSA_HEADS, HEAD_DIM), kv(nkc), kv(nvc), kv(nks), kv(nvs), kv(nkw), kv(nvw),
                        heads(ngate, NSA_HEADS, 3), nsa_q_norm, nsa_k_norm, nsa_cmp_pe, nsa_cmp_w1, nsa_cmp_w2,
                        cos, sin)
    o_c = dsa_attention(heads(dq, DSA_HEADS, HEAD_DIM), dk, dv, heads(iq, DSA_IDX_HEADS, DSA_IDX_DIM), ik, iw,
                        dsa_q_norm, dsa_k_norm, cos, sin, cos_i, sin_i)

    g_a, g_b, g_c = jnp.split(jax.nn.sigmoid(h @ w_gate), 3, axis=-1)
    merged = g_a * (o_a @ w_br_a) + g_b * (o_b @ w_br_b) + g_c * (o_c @ w_br_c)
    return merged @ w_out


def setup_inputs(seed: int = 0) -> dict:
    key = jax.random.key(seed)
    ks = jax.random.split(key, 21)
    f32 = jnp.float32

    def normal(k, shape, scale):
        return jax.random.normal(k, shape, f32) * scale

    def gain(k, shape):
        return 1.0 + 0.02 * jax.random.normal(k, shape, f32)

    D, L = D_MODEL, DEPTH
    cmp_in = NSA_CMP_LEN * HEAD_DIM
    return {
        "x": jax.random.normal(ks[0], (BATCH, SEQ, D), f32),
        "ffn1_norm": gain(ks[1], (L, D)),
        "ffn1_w_gu": normal(ks[2], (L, D, 2 * D_FF), D ** -0.5),
        "ffn1_w_down": normal(ks[3], (L, D_FF, D), D_FF ** -0.5),
        "mix_norm": gain(ks[4], (L, D)),
        "w_in": normal(ks[5], (L, D, D_IN), D ** -0.5),
        "w_gate": normal(ks[6], (L, D, 3 * D), D ** -0.5),
        "nsa_q_norm": gain(ks[7], (L, HEAD_DIM)),
        "nsa_k_norm": gain(ks[8], (L, 3, HEAD_DIM)),
        "nsa_cmp_pe": normal(ks[9], (L, 2, NSA_CMP_LEN, HEAD_DIM), 0.1),
        "nsa_cmp_w1": normal(ks[10], (L, 2, cmp_in, NSA_CMP_HIDDEN), cmp_in ** -0.5),
        "nsa_cmp_w2": normal(ks[11], (L, 2, NSA_CMP_HIDDEN, HEAD_DIM), NSA_CMP_HIDDEN ** -0.5),
        "dsa_q_norm": gain(ks[12], (L, HEAD_DIM)),
        "dsa_k_norm": gain(ks[13], (L, HEAD_DIM)),
        "w_br_a": normal(ks[14], (L, SB_W, D), SB_W ** -0.5),
        "w_br_b": normal(ks[15], (L, NSA_QW, D), NSA_QW ** -0.5),
        "w_br_c": normal(ks[16], (L, DSA_QW, D), DSA_QW ** -0.5),
        "w_out": normal(ks[17], (L, D, D), D ** -0.5),
        "ffn2_norm": gain(ks[18], (L, D)),
        "ffn2_w_gu": normal(ks[19], (L, D, 2 * D_FF), D ** -0.5),
        "ffn2_w_down": normal(ks[20], (L, D_FF, D), D_FF ** -0.5),
    }


def reference(x, ffn1_norm, ffn1_w_gu, ffn1_w_down, mix_norm, w_in, w_gate, nsa_q_norm, nsa_k_norm,
              nsa_cmp_pe, nsa_cmp_w1, nsa_cmp_w2, dsa_q_norm, dsa_k_norm, w_br_a, w_br_b, w_br_c, w_out,
              ffn2_norm, ffn2_w_gu, ffn2_w_down):
    S = x.shape[1]
    pos = jnp.arange(S)
    cos, sin = rope_tables(pos, ROT_DIM)
    cos_i, sin_i = rope_tables(pos, DSA_IDX_ROT)
    for l in range(DEPTH):
        x = x + 0.5 * swiglu_ffn(x, ffn1_norm[l], ffn1_w_gu[l], ffn1_w_down[l])
        h = rms_norm(x, mix_norm[l])
        x = x + hybrid_mixer(h, w_in[l], w_gate[l], nsa_q_norm[l], nsa_k_norm[l], nsa_cmp_pe[l],
                             nsa_cmp_w1[l], nsa_cmp_w2[l], dsa_q_norm[l], dsa_k_norm[l],
                             w_br_a[l], w_br_b[l], w_br_c[l], w_out[l], cos, sin, cos_i, sin_i)
        x = x + 0.5 * swiglu_ffn(x, ffn2_norm[l], ffn2_w_gu[l], ffn2_w_down[l])
    return x
```

```python
import numpy as np
import ml_dtypes
from contextlib import ExitStack
import concourse.bass as bass
import concourse.mybir as mybir
from concourse.bass_utils import run_bass_kernel_spmd

F32 = mybir.dt.float32
BF16 = mybir.dt.bfloat16
AF = mybir.ActivationFunctionType
ALU = mybir.AluOpType
AX = mybir.AxisListType

D = 1024
DFF = 2816
NFF = DFF // 128
DIN = 2752
EPS = 1e-6
NEG = -1.0e30


class Buf:
    __slots__ = ("t", "name", "lw", "rd")

    def __init__(self, t, name):
        self.t = t
        self.name = name
        self.lw = None
        self.rd = {}

    def __getitem__(self, k):
        return self.t[k]


class Prog:
    ENG = ("pe", "act", "dve", "pool", "sp")

    def __init__(self):
        self.nc = bass.Bass("TRN2", target_bir_lowering=False)
        self.es = ExitStack()
        self.q = {e: [] for e in self.ENG}
        self.cnt = {e: 0 for e in self.ENG}
        self.dcnt = {}
        self.sems = {}
        self.known = {e: {} for e in self.ENG}
        self.nbuf = 0
        self.outs = []
        self.n_ops = 0
        nc = self.nc
        self.eobj = {"pe": nc.tensor, "act": nc.scalar, "dve": nc.vector, "pool": nc.gpsimd, "sp": nc.sync}
        self.scopes = []

    def _sem(self, key):
        if key not in self.sems:
            self.sems[key] = self.es.enter_context(self.nc.semaphore("s_" + key))
        return self.sems[key]

    def push(self):
        self.scopes.append(ExitStack())

    def pop(self):
        self.barrier()
        self.scopes.pop().close()

    def barrier(self):
        pts = [("E_" + e, self.cnt[e]) for e in self.ENG if self.cnt[e] > 0] + list(self.dcnt.items())
        for e in self.ENG:
            for k, v in pts:
                if k == "E_" + e:
                    continue
                if self.known[e].get(k, 0) < v:
                    self.known[e][k] = v
                    self.eobj[e].wait_ge(self._sem(k), v)

    def sbuf(self, name, shape, dt):
        self.nbuf += 1
        t = (self.scopes[-1] if self.scopes else self.es).enter_context(self.nc.sbuf_tensor("sb_" + name, list(shape), dt))
        return Buf(t, name)

    def psum(self, name, shape, dt):
        t = self.es.enter_context(self.nc.psum_tensor("ps_" + name, list(shape), dt))
        return Buf(t, name)

    def dram(self, name, shape, dt, kind):
        t = self.nc.dram_tensor(name, list(shape), dt, kind=kind).ap()
        b = Buf(t, name)
        if kind == "ExternalOutput":
            self.outs.append(b)
        return b

    def _deps(self, eng, reads, writes):
        waits = {}

        def need(k, v):
            if eng == "pe" and k == "E_pe":
                return
            if self.known[eng].get(k, 0) >= v:
                return
            if waits.get(k, 0) < v:
                waits[k] = v

        for b in reads:
            if b.lw is not None:
                need(*b.lw)
        for b in writes:
            if b.lw is not None:
                need(*b.lw)
            for k, v in b.rd.items():
                need(k, v)
        for k, v in waits.items():
            self.known[eng][k] = v
            self.eobj[eng].wait_ge(self._sem(k), v)

    def _done(self, pt, reads, writes):
        k, v = pt
        for b in reads:
            if b.rd.get(k, 0) < v:
                b.rd[k] = v
        for b in writes:
            b.lw = pt
            b.rd = {}

    def op(self, eng, fn, reads=(), writes=()):
        self._deps(eng, reads, writes)
        self.cnt[eng] += 1
        k = "E_" + eng
        fn(self.eobj[eng]).then_inc(self._sem(k), 1)
        self._done((k, self.cnt[eng]), reads, writes)
        self.n_ops += 1

    def dma(self, eng, out_buf, out_ap, in_buf, in_ap, sem_buf=None, **kw):
        self._deps(eng, (in_buf,), (out_buf,))
        sb = sem_buf if sem_buf is not None else out_buf
        k = "D_" + sb.name
        self.dcnt[k] = self.dcnt.get(k, 0) + 16
        self.eobj[eng].dma_start(out=out_ap, in_=in_ap, **kw).then_inc(self._sem(k), 16)
        self._done((k, self.dcnt[k]), (in_buf,), (out_buf,))
        self.n_ops += 1

    def finish(self):
        for b in self.outs:
            self._deps("sp", (b,), (b,))
        self.barrier()
        self.es.close()
        return self.nc


def mm(P, out_b, out_ap, l_b, l_ap, r_b, r_ap, start, stop, extra_reads=()):
    P.op("pe", lambda e: e.matmul(out_ap, l_ap, r_ap, start=start, stop=stop),
         reads=(l_b, r_b) + tuple(extra_reads), writes=(out_b,))


def tr(P, out_b, out_ap, in_b, in_ap, id_b, id_ap):
    P.op("pe", lambda e: e.transpose(out_ap, in_ap, id_ap), reads=(in_b, id_b), writes=(out_b,))


def act(P, out_b, out_ap, in_b, in_ap, func, scale=None, bias=None, extra=(), accum=None, accum_b=None):
    kw = {}
    if scale is not None:
        kw["scale"] = scale
    if bias is not None:
        kw["bias"] = bias
    if accum is not None:
        kw["accum_out"] = accum
    w = (out_b,) + ((accum_b,) if accum_b is not None else ())
    P.op("act", lambda e: e.activation(out_ap, in_ap, func, **kw), reads=(in_b,) + tuple(extra), writes=w)


def tt(P, eng, out_b, out_ap, a_b, a_ap, b_b, b_ap, op):
    P.op(eng, lambda e: e.tensor_tensor(out_ap, a_ap, b_ap, op), reads=(a_b, b_b), writes=(out_b,))


def ts(P, eng, out_b, out_ap, a_b, a_ap, s1, s2, op0, op1=None, extra=(), accum=None, accum_b=None):
    kw = {}
    if op1 is not None:
        kw["op1"] = op1
    if accum is not None:
        kw["accum_out"] = accum
    w = (out_b,) + ((accum_b,) if accum_b is not None else ())
    P.op(eng, lambda e: e.tensor_scalar(out_ap, a_ap, s1, s2, op0, **kw), reads=(a_b,) + tuple(extra), writes=w)


def stt(P, out_b, out_ap, a_b, a_ap, scalar, b_b, b_ap, op0, op1, extra=(), accum=None, accum_b=None):
    kw = {}
    if accum is not None:
        kw["accum_out"] = accum
    w = (out_b,) + ((accum_b,) if accum_b is not None else ())
    P.op("dve", lambda e: e.scalar_tensor_tensor(out_ap, a_ap, scalar, b_ap, op0, op1, **kw),
         reads=(a_b, b_b) + tuple(extra), writes=w)


def cp(P, eng, out_b, out_ap, in_b, in_ap):
    if eng == "act":
        P.op("act", lambda e: e.copy(out_ap, in_ap), reads=(in_b,), writes=(out_b,))
    else:
        P.op(eng, lambda e: e.tensor_copy(out_ap, in_ap), reads=(in_b,), writes=(out_b,))


def mset(P, eng, b, ap, val):
    P.op(eng, lambda e: e.memset(ap, val), reads=(), writes=(b,))


def load_w(P, dst, src, src_rows0, nrows, col0, ncols, dst_col0=0):
    nk = nrows // 128
    for kc in range(nk):
        for c0 in range(0, ncols, 2048):
            cw = min(2048, ncols - c0)
            r0 = src_rows0 + kc * 128
            P.dma("pool", dst, dst[:, kc, dst_col0 + c0:dst_col0 + c0 + cw],
                  src, src[r0:r0 + 128, col0 + c0:col0 + c0 + cw])


class Ctx:
    pass


def make_ctx(P, ident_d):
    C = Ctx()
    C.ident = P.sbuf("ident", [128, 128], BF16)
    P.dma("sp", C.ident, C.ident[:], ident_d, ident_d[:, :])
    C.banks = [P.psum("bank%d" % i, [128, 512], F32) for i in range(8)]
    return C


def bank_bf(b):
    return b.t[:].bitcast(BF16)


def norm_to_hT(P, C, xt, G, gT, hT, ss, rstd, hbf, tpb, junk):
    for t in range(G):
        stt(P, junk, junk[:], xt[t], xt[t][:], 1.0, xt[t], xt[t][:], ALU.mult, ALU.mult,
            accum=ss[:, t:t + 1], accum_b=ss)
    ts(P, "dve", rstd, rstd[:, 0:G], ss, ss[:, 0:G], 1.0 / D, EPS, ALU.mult, ALU.add)
    act(P, rstd, rstd[:, 0:G], rstd, rstd[:, 0:G], AF.Sqrt)
    P.op("dve", lambda e: e.reciprocal(rstd[:, 0:G], rstd[:, 0:G]), reads=(rstd,), writes=(rstd,))
    for t in range(G):
        act(P, hbf[t], hbf[t][:], xt[t], xt[t][:], AF.Copy, scale=rstd[:, t:t + 1], extra=(rstd,))
    for kc in range(8):
        pb = tpb[kc % 2]
        pv = bank_bf(pb)
        for t in range(G):
            tr(P, pb, pv[:, t * 128:(t + 1) * 128], hbf[t], hbf[t][:, kc * 128:(kc + 1) * 128], C.ident, C.ident[:])
        ts(P, "dve", hT, hT[:, kc, 0:G * 128], pb, pv[:, 0:G * 128], gT[:, kc:kc + 1], None, ALU.mult, extra=(gT,))


def ffn_pass(P, C, x_in, x_out, g_d, wgu_d, wd_d, NT, G, tag):
    GT = G * 128
    wgu = P.sbuf("wgu" + tag, [128, 8, 2 * DFF], BF16)
    wd = P.sbuf("wd" + tag, [128, NFF, D], BF16)
    gT = P.sbuf("gT" + tag, [128, 8], F32)
    load_w(P, wgu, wgu_d, 0, D, 0, 2 * DFF)
    load_w(P, wd, wd_d, 0, DFF, 0, D)
    P.dma("sp", gT, gT[:], g_d, g_d.t.rearrange("(k p) -> p k", p=128), allow_slow_non_contiguous=True)
    xt = [P.sbuf("xt%s%d" % (tag, t), [128, D], F32) for t in range(G)]
    hbf = [P.sbuf("hbf%s%d" % (tag, t), [128, D], BF16) for t in range(G)]
    hT = P.sbuf("hT" + tag, [128, 8, GT], BF16)
    hid = P.sbuf("hid" + tag, [128, NFF, GT], BF16)
    sg = [P.sbuf("sg%s%d" % (tag, i), [128, GT], BF16) for i in range(2)]
    ss = P.sbuf("ss" + tag, [128, 8], F32)
    rstd = P.sbuf("rstd" + tag, [128, 8], F32)
    junk = P.sbuf("junk" + tag, [128, D], BF16)
    B = C.banks
    for g in range(NT // GT):
        for t in range(G):
            r0 = g * GT + t * 128
            P.dma("sp", xt[t], xt[t][:], x_in, x_in[r0:r0 + 128, :])
        norm_to_hT(P, C, xt, G, gT, hT, ss, rstd, hbf, (B[0], B[1]), junk)
        for c in range(NFF):
            gp = B[2 + (c % 2)]
            up = B[4 + (c % 2)]
            for kc in range(8):
                mm(P, gp, gp[:, 0:GT], wgu, wgu[:, kc, c * 128:(c + 1) * 128], hT, hT[:, kc, :], kc == 0, kc == 7)
            for kc in range(8):
                mm(P, up, up[:, 0:GT], wgu, wgu[:, kc, DFF + c * 128:DFF + (c + 1) * 128], hT, hT[:, kc, :],
                   kc == 0, kc == 7)
            s = sg[c % 2]
            act(P, s, s[:], gp, gp[:, 0:GT], AF.Silu)
            tt(P, "dve", hid, hid[:, c, :], s, s[:], up, up[:, 0:GT], ALU.mult)
        for t in range(G):
            for hf in range(2):
                yp = B[6 + hf]
                for c in range(NFF):
                    mm(P, yp, yp[:, :], hid, hid[:, c, t * 128:(t + 1) * 128], wd, wd[:, c, hf * 512:(hf + 1) * 512],
                       c == 0, c == NFF - 1)
                stt(P, xt[t], xt[t][:, hf * 512:(hf + 1) * 512], yp, yp[:, :], 0.5,
                    xt[t], xt[t][:, hf * 512:(hf + 1) * 512], ALU.mult, ALU.add)
            r0 = g * GT + t * 128
            P.dma("sp", x_out, x_out[r0:r0 + 128, :], xt[t], xt[t][:], sem_buf=xt[t])


PROJ_OUT = {
    "QT_sb": (lambda NT: [64, 4, NT], BF16), "KT_sb": (lambda NT: [64, 4, NT], BF16),
    "V_sb": (lambda NT: [NT, 256], BF16),
    "KCT": (lambda NT: [64, 2, NT], BF16), "VCT": (lambda NT: [64, 2, NT], BF16),
    "NQT": (lambda NT: [64, 8, NT], BF16), "NKT": (lambda NT: [64, 4, NT], BF16),
    "NVS": (lambda NT: [NT, 2, 64], BF16), "NVW": (lambda NT: [NT, 2, 64], BF16),
    "NG": (lambda NT: [NT, 24], F32),
    "DQT": (lambda NT: [64, 5, NT], BF16), "DV": (lambda NT: [NT, 64], BF16),
    "IQT": (lambda NT: [32, 8, NT], BF16), "IKT": (lambda NT: [32, NT], BF16),
    "SGN": (lambda NT: [NT, 8], F32),
    "GT": (lambda NT: [24, 128, NT], BF16),
}


def hv(ap, d):
    return ap.rearrange("p (h d) -> p h d", d=d)


def proj_pass(P, C, x_in, g_d, win_d, wgate_d, gains_d, rope_d, O, NT, G, tag):
    GT = G * 128
    win = P.sbuf("win" + tag, [128, 8, DIN], BF16)
    wg = P.sbuf("wgate" + tag, [128, 8, 3 * D], BF16)
    gT = P.sbuf("pgT" + tag, [128, 8], F32)
    load_w(P, win, win_d, 0, D, 0, DIN)
    load_w(P, wg, wgate_d, 0, D, 0, 3 * D)
    P.dma("sp", gT, gT[:], g_d, g_d.t.rearrange("(k p) -> p k", p=128), allow_slow_non_contiguous=True)
    gains = P.sbuf("gains" + tag, [128, 5, 64], F32)
    for r in range(5):
        P.dma("sp", gains, gains[:, r, :], gains_d, gains_d[r:r + 1, :].broadcast_to([128, 64]))
    ts(P, "dve", gains, gains[:, 0, :], gains, gains[:, 0, :], 0.125, None, ALU.mult)
    ts(P, "dve", gains, gains[:, 3, :], gains, gains[:, 3, :], 0.125, None, ALU.mult)
    xt = [P.sbuf("pxt%s%d" % (tag, t), [128, D], F32) for t in range(G)]
    hbf = [P.sbuf("phbf%s%d" % (tag, t), [128, D], BF16) for t in range(G)]
    hT = P.sbuf("phT" + tag, [128, 8, GT], BF16)
    ss = P.sbuf("pss" + tag, [128, 8], F32)
    rstd = P.sbuf("prstd" + tag, [128, 8], F32)
    junk = P.sbuf("pjunk" + tag, [128, D], BF16)
    stg = [P.sbuf("pstg%s%d" % (tag, i), [128, GT], BF16) for i in range(3)]
    nat = [P.sbuf("nat%s%d" % (tag, i), [128, DIN], F32) for i in range(2)]
    sq = [P.sbuf("sq%s%d" % (tag, i), [128, 1088], F32) for i in range(1)]
    tmb = [P.sbuf("tmb%s%d" % (tag, i), [128, DIN], BF16) for i in range(1)]
    rp = [P.sbuf("rp%s%d" % (tag, i), [128, 24], F32) for i in range(2)]
    sm = [P.sbuf("sm%s%d" % (tag, i), [128, 64], F32) for i in range(2)]
    tmpr = [P.sbuf("tmpr%s%d" % (tag, i), [128, 4, 9, 8], F32) for i in range(2)]
    sgn = [P.sbuf("sgn%s%d" % (tag, i), [128, 8], F32) for i in range(2)]
    ngt = [P.sbuf("ngt%s%d" % (tag, i), [128, 24], F32) for i in range(2)]
    st_q = P.sbuf("stq" + tag, [64, 8, GT], BF16)
    st_k = P.sbuf("stk" + tag, [64, 4, GT], BF16)
    st_d = P.sbuf("std" + tag, [64, 5, GT], BF16)
    st_i = P.sbuf("sti" + tag, [32, 8, GT], BF16)
    st_ik = P.sbuf("stik" + tag, [32, GT], BF16)
    B = C.banks
    direct = [(0, "QT_sb", 0, -0.125), (128, "QT_sb", 1, -0.125), (256, "KT_sb", 0, 1.0), (384, "KT_sb", 1, 1.0),
              (1280, "KCT", 0, 1.0), (1408, "VCT", 0, 1.0)]
    natch = [(512, 1024), (1024, 1280), (1536, 2048), (2048, 2560), (2560, 2752)]
    norms = [(768, 8, 0, 0), (1536, 2, 8, 1), (1792, 2, 10, 2), (2072, 4, 12, 3), (2328, 1, 16, 4)]
    ropes = [(768, 8, 64, 8, 0), (1536, 2, 64, 8, 0), (1792, 2, 64, 8, 0), (2072, 5, 64, 8, 0), (2456, 9, 32, 4, 16)]
    nstg = 0
    for g in range(NT // GT):
        tok = slice(g * GT, (g + 1) * GT)
        for t in range(G):
            r0 = g * GT + t * 128
            P.dma("sp", xt[t], xt[t][:], x_in, x_in[r0:r0 + 128, :])
        norm_to_hT(P, C, xt, G, gT, hT, ss, rstd, hbf, (B[0], B[1]), junk)
        for i, (c0, name, idx, sc) in enumerate(direct):
            pb = B[2 + (i % 2)]
            for kc in range(8):
                mm(P, pb, pb[:, 0:GT], win, win[:, kc, c0:c0 + 128], hT, hT[:, kc, :], kc == 0, kc == 7)
            s = stg[nstg % 3]
            nstg += 1
            act(P, s, s[:], pb, pb[:, 0:GT], AF.Copy, scale=sc)
            dst = O[name]
            if idx is not None:
                P.dma("sp", dst, dst[:, 2 * idx, tok], s, s[0:64, :], sem_buf=s)
                P.dma("sp", dst, dst[:, 2 * idx + 1, tok], s, s[64:128, :], sem_buf=s)
            else:
                P.dma("sp", dst, dst[:, tok], s, s[:], sem_buf=s)
        for ch in range(24):
            pb = B[2 + (ch % 2)]
            for kc in range(8):
                mm(P, pb, pb[:, 0:GT], wg, wg[:, kc, ch * 128:(ch + 1) * 128], hT, hT[:, kc, :], kc == 0, kc == 7)
            s = stg[nstg % 3]
            nstg += 1
            act(P, s, s[:], pb, pb[:, 0:GT], AF.Sigmoid)
            P.dma("sp", O["GT"], O["GT"][ch, :, tok], s, s[:], sem_buf=s)
        for t in range(G):
            r0 = g * GT + t * 128
            rows = slice(r0, r0 + 128)
            n_, q_, b_, rp_, sm_, tp_ = nat[t % 2], sq[0], tmb[0], rp[t % 2], sm[t % 2], tmpr[t % 2]
            P.dma("sp", rp_, rp_[:], rope_d, rope_d[rows, :])
            for i, (a, b) in enumerate(natch):
                pb = B[4 + (i % 2)]
                for kc in range(8):
                    mm(P, pb, pb[:, 0:b - a], hT, hT[:, kc, t * 128:(t + 1) * 128], win, win[:, kc, a:b],
                       kc == 0, kc == 7)
                if i % 2 == 0:
                    cp(P, "act", n_, n_[:, a:b], pb, pb[:, 0:b - a])
                else:
                    cp(P, "dve", n_, n_[:, a:b], pb, pb[:, 0:b - a])
            for (off, H, sc0, gr) in norms:
                w = H * 64
                qo = sc0 * 64
                tt(P, "pool", q_, q_[:, qo:qo + w], n_, n_[:, off:off + w], n_, n_[:, off:off + w], ALU.mult)
                P.op("dve", lambda e, o_=sm_[:, sc0:sc0 + H], i_=hv(q_[:, qo:qo + w], 64):
                     e.tensor_reduce(o_, i_, AX.X, ALU.add), reads=(q_,), writes=(sm_,))
            ts(P, "dve", sm_, sm_[:, 17:34], sm_, sm_[:, 0:17], 1.0 / 64, EPS, ALU.mult, ALU.add)
            act(P, sm_, sm_[:, 17:34], sm_, sm_[:, 17:34], AF.Sqrt)
            P.op("dve", lambda e, a_=sm_[:, 17:34]: e.reciprocal(a_, a_), reads=(sm_,), writes=(sm_,))
            for (off, H, sc0, gr) in norms:
                w = H * 64
                v = hv(n_[:, off:off + w], 64)
                tt(P, "dve", n_, v, n_, v, sm_, sm_[:, 17 + sc0:17 + sc0 + H].unsqueeze(2).broadcast_to([128, H, 64]),
                   ALU.mult)
                tt(P, "dve", n_, v, n_, v, gains, gains[:, gr:gr + 1, :].broadcast_to([128, H, 64]), ALU.mult)
            for (off, H, hd, r, tc) in ropes:
                v = hv(n_[:, off:off + H * hd], hd)
                x1, x2 = v[:, :, 0:r], v[:, :, r:2 * r]
                cs = rp_[:, tc:tc + r].unsqueeze(1).broadcast_to([128, H, r])
                sn = rp_[:, tc + r:tc + 2 * r].unsqueeze(1).broadcast_to([128, H, r])
                t1, t2, t3, t4 = (tp_[:, j, 0:H, 0:r] for j in range(4))
                tt(P, "dve", tp_, t1, n_, x1, rp_, cs, ALU.mult)
                tt(P, "dve", tp_, t2, n_, x2, rp_, sn, ALU.mult)
                tt(P, "dve", tp_, t3, n_, x2, rp_, cs, ALU.mult)
                tt(P, "dve", tp_, t4, n_, x1, rp_, sn, ALU.mult)
                tt(P, "dve", n_, x1, tp_, t1, tp_, t2, ALU.subtract)
                tt(P, "dve", n_, x2, tp_, t3, tp_, t4, ALU.add)
            act(P, sm_, sm_[:, 40:48], n_, n_[:, 2744:2752], AF.Abs, scale=1.0 / 16)
            vq = hv(n_[:, 2456:2712], 32)
            tt(P, "dve", n_, vq, n_, vq, sm_, sm_[:, 40:48].unsqueeze(2).broadcast_to([128, 8, 32]), ALU.mult)
            act(P, sgn[t % 2], sgn[t % 2][:], n_, n_[:, 2744:2752], AF.Sign)
            P.dma("sp", O["SGN"], O["SGN"][rows, :], sgn[t % 2], sgn[t % 2][:], sem_buf=sgn[t % 2])
            act(P, ngt[t % 2], ngt[t % 2][:], n_, n_[:, 2048:2072], AF.Sigmoid)
            P.dma("sp", O["NG"], O["NG"][rows, :], ngt[t % 2], ngt[t % 2][:], sem_buf=ngt[t % 2])
            cp(P, "act", b_, b_[:, 512:1280], n_, n_[:, 512:1280])
            cp(P, "pool", b_, b_[:, 1536:2752], n_, n_[:, 1536:2752])
            P.dma("sp", O["V_sb"], O["V_sb"][rows, :], b_, b_[:, 512:768], sem_buf=b_)
            P.dma("sp", O["NVS"], O["NVS"][rows, :, :], b_, hv(b_[:, 1664:1792], 64), sem_buf=b_)
            P.dma("sp", O["NVW"], O["NVW"][rows, :, :], b_, hv(b_[:, 1920:2048], 64), sem_buf=b_)
            P.dma("sp", O["DV"], O["DV"][rows, :], b_, b_[:, 2392:2456], sem_buf=b_)
            tcol = slice(t * 128, (t + 1) * 128)
            pv6, pv7 = bank_bf(B[6]), bank_bf(B[7])
            for h in range(8):
                tr(P, B[6], pv6[0:64, h * 128:(h + 1) * 128], b_, b_[:, 768 + h * 64:768 + (h + 1) * 64],
                   C.ident, C.ident[:])
            cp(P, "act", st_q, st_q[:, :, tcol], B[6], hv(pv6[0:64, 0:1024], 128))
            for j, off in enumerate((1536, 1600, 1792, 1856)):
                tr(P, B[7], pv7[0:64, j * 128:(j + 1) * 128], b_, b_[:, off:off + 64], C.ident, C.ident[:])
            cp(P, "dve", st_k, st_k[:, :, tcol], B[7], hv(pv7[0:64, 0:512], 128))
            for j in range(5):
                off = 2072 + j * 64
                tr(P, B[6], pv6[0:64, j * 128:(j + 1) * 128], b_, b_[:, off:off + 64], C.ident, C.ident[:])
            tr(P, B[6], pv6[0:32, 640:768], b_, b_[:, 2712:2744], C.ident, C.ident[:])
            cp(P, "act", st_d, st_d[:, :, tcol], B[6], hv(pv6[0:64, 0:640], 128))
            cp(P, "act", st_ik, st_ik[:, tcol], B[6], pv6[0:32, 640:768])
            for h in range(8):
                off = 2456 + h * 32
                tr(P, B[7], pv7[0:32, h * 128:(h + 1) * 128], b_, b_[:, off:off + 32], C.ident, C.ident[:])
            cp(P, "dve", st_i, st_i[:, :, tcol], B[7], hv(pv7[0:32, 0:1024], 128))
        P.dma("sp", O["NQT"], O["NQT"][:, :, tok], st_q, st_q[:], sem_buf=st_q)
        P.dma("sp", O["NKT"], O["NKT"][:, :, tok], st_k, st_k[:], sem_buf=st_k)
        P.dma("sp", O["DQT"], O["DQT"][:, :, tok], st_d, st_d[:], sem_buf=st_d)
        P.dma("sp", O["IQT"], O["IQT"][:, :, tok], st_i, st_i[:], sem_buf=st_i)
        P.dma("sp", O["IKT"], O["IKT"][:, tok], st_ik, st_ik[:], sem_buf=st_ik)


def sb_pass(P, C, QT_d, KT_d, V_d, OA_d, K_, S, NTL, tag="sb"):
    P.push()
    nblk = S // 128
    KT = P.sbuf("sbKT", [64, 4, S], BF16)
    V = P.sbuf("sbV", [128, nblk, 256], BF16)
    for i in range(4):
        for c0 in range(0, S, 1024):
            P.dma("sp", KT, KT[:, i, c0:c0 + 1024], KT_d, KT_d[:, i, c0:c0 + 1024])
    for j0 in range(0, nblk, 8):
        P.dma("sp", V, V[:, j0:j0 + 8, :], V_d, V_d[j0 * 128:(j0 + 8) * 128, :].rearrange("(j p) c -> p j c", p=128))
    q = [P.sbuf("sbq%d" % i, [64, 4, 128], BF16) for i in range(2)]
    Et = [P.sbuf("sbE%d" % i, [128, 512], F32) for i in range(2)]
    nLb = [P.sbuf("sbnL%d" % i, [128, 512], BF16) for i in range(2)]
    wb = [P.sbuf("sbw%d" % i, [128, 512], BF16) for i in range(2)]
    Cacc = P.sbuf("sbCacc", [128, 512], F32)
    Cbf = [P.sbuf("sbCbf%d" % i, [128, 512], BF16) for i in range(2)]
    ob = [P.sbuf("sbo%d" % i, [128, 256], BF16) for i in range(2)]
    B = C.banks
    sbm = K_["sbm"]
    nb = 0
    for m in range(NTL // 128):
        J = 2 * m + 2
        qq = q[m % 2]
        P.dma("sp", qq, qq[:], QT_d, QT_d[:, :, m * 128:(m + 1) * 128])
        mset(P, "pool", Cacc, Cacc[:], 0.0)
        O = B[4 + (m % 2)]
        for idx in range(J):
            j = J - 1 - idx
            A, Bk = B[nb % 2], B[2 + (nb % 2)]
            E_, n_, w_ = Et[nb % 2], nLb[nb % 2], wb[nb % 2]
            nb += 1
            ks = slice(j * 128, (j + 1) * 128)
            for h in range(4):
                mm(P, A, A[:, h * 128:(h + 1) * 128], KT, KT[:, h, ks], qq, qq[:, h, :], h == 0, h == 3)
            act(P, E_, E_[:], A, A[:, :], AF.Exp, scale=-1.0)
            act(P, n_, n_[:], E_, E_[:], AF.Ln, bias=1.0)
            if idx < 2:
                mk = sbm[:, 1 - idx:2 - idx, :].broadcast_to([128, 4, 128])
                tt(P, "dve", n_, hv(n_[:], 128), n_, hv(n_[:], 128), sbm, mk, ALU.mult)
            for h in range(4):
                mm(P, Bk, Bk[:, h * 128:(h + 1) * 128], KT, KT[:, h, ks], qq, qq[:, h, :], h == 0, False)
            mm(P, Bk, Bk[:, :], K_["uincl"], K_["uincl"][:], n_, n_[:], False, idx == 0)
            if idx > 0:
                cb = Cbf[idx % 2]
                mm(P, Bk, Bk[:, :], K_["ones"], K_["ones"][:], cb, cb[:], False, True)
            act(P, w_, w_[:], Bk, Bk[:, :], AF.Exp, scale=-1.0)
            if idx < 2:
                mk = sbm[:, 1 - idx:2 - idx, :].broadcast_to([128, 4, 128])
                tt(P, "dve", w_, hv(w_[:], 128), w_, hv(w_[:], 128), sbm, mk, ALU.mult)
            for h in range(4):
                mm(P, O, O[:, h * 64:(h + 1) * 64], w_, w_[:, h * 128:(h + 1) * 128], V, V[:, j, h * 64:(h + 1) * 64],
                   idx == 0 and h == 0, idx == J - 1 and h == 3)
            if idx < J - 1:
                tt(P, "pool", Cacc, Cacc[:], Cacc, Cacc[:], n_, n_[:], ALU.add)
                cb = Cbf[(idx + 1) % 2]
                cp(P, "pool", cb, cb[:], Cacc, Cacc[:])
        o_ = ob[m % 2]
        cp(P, "dve", o_, o_[:], O, O[:, 0:256])
        P.dma("sp", OA_d, OA_d[m * 128:(m + 1) * 128, :], o_, o_[:], sem_buf=o_)
    P.pop()


def host_consts(parity):
    bf = ml_dtypes.bfloat16
    r = np.arange(128)
    K = {}
    K["ident"] = np.eye(128).astype(bf)
    K["uincl"] = (r[:, None] >= r[None, :]).astype(bf)
    K["ones"] = np.ones((128, 128), bf)
    tri_strict = (r[:, None] < r[None, :]).astype(np.float32)
    tri_incl = (r[:, None] <= r[None, :]).astype(np.float32)
    ones = np.ones((128, 128), np.float32)
    zeros = np.zeros((128, 128), np.float32)
    if parity == 0:
        K["sbm"] = np.stack([tri_strict, zeros], 1).astype(bf)
        K["cim"] = np.stack([tri_incl, zeros], 1).astype(bf)
    else:
        K["sbm"] = np.stack([ones, tri_strict], 1).astype(bf)
        K["cim"] = np.stack([ones, tri_incl], 1).astype(bf)
    return K


def nsa_pass(P, C, D_, K_, S, NTL):
    P.push()
    nblk = S // 128
    NC4 = (S // 16 + 127) // 128
    NCP = NC4 * 128
    B = C.banks
    ident = C.ident
    NKT = P.sbuf("nNKT", [64, 4, S], BF16)
    for i in range(4):
        for c0 in range(0, S, 1024):
            P.dma("sp", NKT, NKT[:, i, c0:c0 + 1024], D_["NKT"], D_["NKT"][:, i, c0:c0 + 1024])
    vse = P.sbuf("nvse", [128, nblk, 2, 65], BF16)
    vwe = P.sbuf("nvwe", [128, nblk, 2, 65], BF16)
    for (dst, src) in ((vse, D_["NVS"]), (vwe, D_["NVW"])):
        mset(P, "pool", dst, dst[:], 1.0)
        for j0 in range(0, nblk, 8):
            for g in range(2):
                P.dma("sp", dst, dst[:, j0:j0 + 8, g, 0:64], src,
                      src[j0 * 128:(j0 + 8) * 128, g, :].rearrange("(j p) d -> p j d", p=128))
    kcT = P.sbuf("nkcT", [64, 2, NCP], BF16)
    vce = P.sbuf("nvce", [128, NC4, 2, 193], BF16)
    mset(P, "pool", vce, vce[:], 1.0)
    for jc in range(NC4):
        for g in range(2):
            P.dma("sp", vce, vce[:, jc, g, 65:193], K_["overlap"], K_["overlap"][jc * 128:(jc + 1) * 128, :])
    P.push()
    XW = max(S + 16, 16 * NCP + 32)
    xc = P.sbuf("nxc", [64, 2, XW], BF16)
    W1 = P.sbuf("nW1", [64, 32, 128], BF16)
    W2 = P.sbuf("nW2", [128, 64], BF16)
    peT = P.sbuf("npeT", [64, 32], BF16)
    bia = P.sbuf("nbias", [128, 1], F32)
    hTc = P.sbuf("nhTc", [128, NCP], BF16)
    kn0 = P.sbuf("nkn0", [128, 64], F32)
    rpc = P.sbuf("nrpc", [128, NC4, 16], F32)
    kc32 = P.sbuf("nkc32", [128, 64], F32)
    kcsq = P.sbuf("nkcsq", [128, 64], F32)
    kcs = P.sbuf("nkcs", [128, 4], F32)
    kct4 = P.sbuf("nkct4", [128, 4, 8], F32)
    kcb = P.sbuf("nkcb", [128, 64], BF16)
    P.dma("sp", kn0, kn0[:], D_["kn0"], D_["kn0"][0:1, :].broadcast_to([128, 64]))
    P.dma("sp", rpc, rpc[:], D_["ropec"], D_["ropec"].t.rearrange("(j p) c -> p j c", p=128))
    for st, src in enumerate((D_["KCT"], D_["VCT"])):
        mset(P, "pool", xc, xc[:, :, S:XW], 0.0)
        for g in range(2):
            for c0 in range(0, S, 1024):
                P.dma("sp", xc, xc[:, g, c0:c0 + 1024], src, src[:, g, c0:c0 + 1024])
        for l0 in range(0, 32, 8):
            P.dma("pool", W1, W1[:, l0:l0 + 8, :], D_["w1"],
                  D_["w1"][st, l0 * 64:(l0 + 8) * 64, :].rearrange("(l d) n -> d l n", d=64))
        P.dma("pool", W2, W2[:], D_["w2"], D_["w2"][st, :, :])
        P.dma("pool", peT, peT[:], D_["pe"], D_["pe"][st, :, :].rearrange("l d -> d l"), allow_slow_non_contiguous=True)
        bp = B[0]
        for l in range(32):
            mm(P, bp, bp[:, 0:1], W1, W1[:, l, :], peT, peT[:, l:l + 1], l == 0, l == 31)
        cp(P, "dve", bia, bia[:], bp, bp[:, 0:1])
        for g in range(2):
            for cc in range(0, NCP, 512):
                n = min(512, NCP - cc)
                hp = B[1 + (cc // 512) % 2]
                for l in range(32):
                    a0 = cc * 16 + l
                    mm(P, hp, hp[:, 0:n], W1, W1[:, l, :], xc, xc[:, g, a0:a0 + 16 * (n - 1) + 1:16], l == 0, l == 31)
                act(P, hTc, hTc[:, cc:cc + n], hp, hp[:, 0:n], AF.Silu, bias=bia[:, 0:1], extra=(bia,))
            for jc in range(NC4):
                op_ = B[3 + jc % 2]
                mm(P, op_, op_[:, 0:64], hTc, hTc[:, jc * 128:(jc + 1) * 128], W2, W2[:], True, True)
                if st == 1:
                    cp(P, "act", vce, vce[:, jc, g, 0:64], op_, op_[:, 0:64])
                else:
                    cp(P, "act", kc32, kc32[:], op_, op_[:, 0:64])
                    stt(P, kcsq, kcsq[:], kc32, kc32[:], 1.0, kc32, kc32[:], ALU.mult, ALU.mult,
                        accum=kcs[:, 0:1], accum_b=kcs)
                    ts(P, "dve", kcs, kcs[:, 1:2], kcs, kcs[:, 0:1], 1.0 / 64, EPS, ALU.mult, ALU.add)
                    act(P, kcs, kcs[:, 1:2], kcs, kcs[:, 1:2], AF.Sqrt)
                    P.op("dve", lambda e: e.reciprocal(kcs[:, 2:3], kcs[:, 1:2]), reads=(kcs,), writes=(kcs,))
                    stt(P, kc32, kc32[:], kc32, kc32[:], kcs[:, 2:3], kn0, kn0[:], ALU.mult, ALU.mult, extra=(kcs,))
                    x1, x2 = kc32[:, 0:8], kc32[:, 8:16]
                    cs, sn = rpc[:, jc, 0:8], rpc[:, jc, 8:16]
                    tt(P, "dve", kct4, kct4[:, 0, :], kc32, x1, rpc, cs, ALU.mult)
                    tt(P, "dve", kct4, kct4[:, 1, :], kc32, x2, rpc, sn, ALU.mult)
                    tt(P, "dve", kct4, kct4[:, 2, :], kc32, x2, rpc, cs, ALU.mult)
                    tt(P, "dve", kct4, kct4[:, 3, :], kc32, x1, rpc, sn, ALU.mult)
                    tt(P, "dve", kc32, x1, kct4, kct4[:, 0, :], kct4, kct4[:, 1, :], ALU.subtract)
                    tt(P, "dve", kc32, x2, kct4, kct4[:, 2, :], kct4, kct4[:, 3, :], ALU.add)
                    cp(P, "dve", kcb, kcb[:], kc32, kc32[:])
                    tp = B[5]
                    tpv = bank_bf(tp)
                    tr(P, tp, tpv[0:64, 0:128], kcb, kcb[:], ident, ident[:])
                    cp(P, "act", kcT, kcT[:, g, jc * 128:(jc + 1) * 128], tp, tpv[0:64, 0:128])
    P.pop()
    qn = [P.sbuf("nqn%d" % i, [64, 8, 128], BF16) for i in range(2)]
    ng = [P.sbuf("nng%d" % i, [128, 24], F32) for i in range(2)]
    Eb = [P.sbuf("nE%d" % i, [128, 512], BF16) for i in range(3)]
    Pm = [P.sbuf("nPm%d" % i, [128, 512], BF16) for i in range(3)]
    cm = [P.sbuf("ncm%d" % i, [128, 128], BF16) for i in range(2)]
    msk = [P.sbuf("nmsk%d" % i, [128, 128], BF16) for i in range(2)]
    imp = P.sbuf("nimp", [128, 128], F32)
    sc = P.sbuf("nsc", [128, 128], F32)
    sc2 = P.sbuf("nsc2", [128, 128], F32)
    m8 = P.sbuf("nm8", [128, 16], F32)
    selm = [P.sbuf("nselm%d" % i, [128, 128], BF16) for i in range(2)]
    selx = [P.sbuf("nselx%d" % i, [128, S], BF16) for i in range(2)]
    dn = P.sbuf("ndn", [128, 16], F32)
    ob = [P.sbuf("nob%d" % i, [128, 8, 64], F32) for i in range(2)]
    obb = [P.sbuf("nobb%d" % i, [128, 512], BF16) for i in range(2)]
    tmpo = P.sbuf("ntmpo", [128, 4, 64], F32)
    nE = 0
    nS = 0

    def score_exp(kidx, ks, qv):
        nonlocal nE, nS
        sp = B[nS % 2]
        nS += 1
        mm(P, sp, sp[:, :], NKT, NKT[:, kidx, ks], qv[0], qv[1], True, True)
        e_ = Eb[nE % 3]
        p_ = Pm[nE % 3]
        nE += 1
        act(P, e_, e_[:], sp, sp[:, :], AF.Exp)
        return e_, p_

    def combine(accv, bank, g, br, o_, ng_, first):
        ts(P, "dve", dn, dn[:, 0:4], bank, accv[:, :, 64], 1e-30, None, ALU.max)
        P.op("dve", lambda e: e.reciprocal(dn[:, 4:8], dn[:, 0:4]), reads=(dn,), writes=(dn,))
        gv = ng_[:, 12 * g:12 * g + 12].rearrange("p (h b) -> p h b", b=3)[:, :, br]
        tt(P, "dve", dn, dn[:, 8:12], dn, dn[:, 4:8], ng_, gv, ALU.mult)
        cf = dn[:, 8:12].unsqueeze(2).broadcast_to([128, 4, 64])
        ov = o_[:, 4 * g:4 * g + 4, :]
        if first:
            tt(P, "dve", o_, ov, bank, accv[:, :, 0:64], dn, cf, ALU.mult)
        else:
            tt(P, "dve", tmpo, tmpo[:], bank, accv[:, :, 0:64], dn, cf, ALU.mult)
            tt(P, "dve", o_, ov, o_, ov, tmpo, tmpo[:], ALU.add)

    for m in range(NTL // 128):
        J = 2 * m + 2
        q_ = qn[m % 2]
        ng_ = ng[m % 2]
        o_ = ob[m % 2]
        P.dma("sp", q_, q_[:], D_["NQT"], D_["NQT"][:, :, m * 128:(m + 1) * 128])
        P.dma("sp", ng_, ng_[:], D_["NG"], D_["NG"][m * 128:(m + 1) * 128, :])
        for g in range(2):
            qv = (q_, q_[:, 4 * g:4 * g + 4, :].rearrange("p h t -> p (h t)"))
            nch = min(NC4, (16 * m + 14) // 128 + 1)
            aC = (B[4], B[5])
            for jc in range(nch):
                sp = B[nS % 2]
                nS += 1
                mm(P, sp, sp[:, :], kcT, kcT[:, g, jc * 128:(jc + 1) * 128], qv[0], qv[1], True, True)
                e_ = Eb[nE % 3]
                p_ = Pm[nE % 3]
                nE += 1
                act(P, e_, e_[:], sp, sp[:, :], AF.Exp)
                c_ = cm[jc % 2]
                th0 = float(256 * m - 2048 * jc - 31)
                ts(P, "dve", c_, c_[:], K_["M0"], K_["M0"][:], K_["par128"][:, 0:1], th0, ALU.subtract, ALU.is_le,
                   extra=(K_["par128"],))
                tt(P, "dve", p_, hv(p_[:], 128), e_, hv(e_[:], 128), c_, c_[:].unsqueeze(1).broadcast_to([128, 4, 128]),
                   ALU.mult)
                for h in range(4):
                    bk = aC[h // 2]
                    mm(P, bk, bk[:, (h % 2) * 193:(h % 2) * 193 + 193], p_, p_[:, h * 128:(h + 1) * 128],
                       vce, vce[:, jc, g, :], jc == 0 and h % 2 == 0, jc == nch - 1 and h % 2 == 1)
            first = True
            for hb in range(2):
                av = aC[hb][:, 0:386].rearrange("p (h c) -> p h c", c=193)
                ts(P, "dve", dn, dn[:, 2 * hb:2 * hb + 2], aC[hb], av[:, :, 64], 1e-30, None, ALU.max)
            P.op("dve", lambda e: e.reciprocal(dn[:, 4:8], dn[:, 0:4]), reads=(dn,), writes=(dn,))
            for h in range(4):
                av = aC[h // 2][:, (h % 2) * 193:(h % 2) * 193 + 193]
                if h == 0:
                    ts(P, "dve", imp, imp[:], aC[0], av[:, 65:193], dn[:, 4:5], None, ALU.mult, extra=(dn,))
                else:
                    stt(P, imp, imp[:], aC[h // 2], av[:, 65:193], dn[:, 4 + h:5 + h], imp, imp[:], ALU.mult, ALU.add,
                        extra=(dn,))
            gv = ng_[:, 12 * g:12 * g + 12].rearrange("p (h b) -> p h b", b=3)[:, :, 0]
            tt(P, "dve", dn, dn[:, 8:12], dn, dn[:, 4:8], ng_, gv, ALU.mult)
            for h in range(4):
                av = aC[h // 2][:, (h % 2) * 193:(h % 2) * 193 + 193]
                ts(P, "dve", o_, o_[:, 4 * g + h, :], aC[h // 2], av[:, 0:64], dn[:, 8 + h:9 + h], None, ALU.mult,
                   extra=(dn,))
            k0 = 128 - 4 * m
            tt(P, "dve", sc, sc[:], imp, imp[:], K_["keepW"], K_["keepW"][:, k0:k0 + 128], ALU.mult)
            tt(P, "dve", sc, sc[:], sc, sc[:], K_["addW"], K_["addW"][:, k0:k0 + 128], ALU.add)
            mset(P, "dve", sc, sc[:, 0:1], 3.0e4)
            P.op("dve", lambda e: e.max(m8[:, 0:8], sc[:]), reads=(sc,), writes=(m8,))
            P.op("dve", lambda e: e.match_replace(sc2[:], m8[:, 0:8], sc[:], NEG), reads=(sc, m8), writes=(sc2,))
            P.op("dve", lambda e: e.max(m8[:, 8:16], sc2[:]), reads=(sc2,), writes=(m8,))
            sm_ = selm[g]
            ts(P, "dve", sm_, sm_[:], sc, sc[:], m8[:, 15:16], None, ALU.is_ge, extra=(m8,))
            sx_ = selx[g]
            cp(P, "dve", sx_, sx_[:, 0:J * 128].rearrange("p (n d) -> p n d", d=64),
               sm_, sm_[:, 0:2 * J].unsqueeze(2).broadcast_to([128, 2 * J, 64]))
            aS = B[6]
            aSv = aS[:, 0:260].rearrange("p (h c) -> p h c", c=65)
            for j in range(J):
                ks = slice(j * 128, (j + 1) * 128)
                mp = B[2 + j % 2]
                mm(P, mp, mp[:, 0:128], sx_, sx_[:, ks], ident, ident[:], True, True)
                e_, p_ = score_exp(g, ks, qv)
                if j >= 2 * m:
                    k_ = msk[j % 2]
                    tt(P, "dve", k_, k_[:], mp, mp[:, 0:128], K_["cim"], K_["cim"][:, j - 2 * m, :], ALU.mult)
                    tt(P, "dve", p_, hv(p_[:], 128), e_, hv(e_[:], 128), k_,
                       k_[:].unsqueeze(1).broadcast_to([128, 4, 128]), ALU.mult)
                else:
                    tt(P, "dve", p_, hv(p_[:], 128), e_, hv(e_[:], 128), mp,
                       mp[:, 0:128].unsqueeze(1).broadcast_to([128, 4, 128]), ALU.mult)
                for h in range(4):
                    mm(P, aS, aS[:, h * 65:(h + 1) * 65], p_, p_[:, h * 128:(h + 1) * 128], vse, vse[:, j, g, :],
                       j == 0 and h == 0, j == J - 1 and h == 3)
            combine(aSv, aS, g, 1, o_, ng_, False)
            aW = B[7]
            aWv = aW[:, 0:260].rearrange("p (h c) -> p h c", c=65)
            rs_ = [r for r in range(6) if 2 * m - 4 + r >= 0]
            for r in rs_:
                j = 2 * m - 4 + r
                ks = slice(j * 128, (j + 1) * 128)
                e_, p_ = score_exp(2 + g, ks, qv)
                tt(P, "dve", p_, hv(p_[:], 128), e_, hv(e_[:], 128), K_["wmask"],
                   K_["wmask"][:, r:r + 1, :].broadcast_to([128, 4, 128]), ALU.mult)
                for h in range(4):
                    mm(P, aW, aW[:, h * 65:(h + 1) * 65], p_, p_[:, h * 128:(h + 1) * 128], vwe, vwe[:, j, g, :],
                       r == rs_[0] and h == 0, r == rs_[-1] and h == 3)
            combine(aWv, aW, g, 2, o_, ng_, False)
        b_ = obb[m % 2]
        cp(P, "act", b_, b_[:], o_, o_[:].rearrange("p h d -> p (h d)"))
        P.dma("sp", D_["OB"], D_["OB"][m * 128:(m + 1) * 128, :], b_, b_[:], sem_buf=b_)
    P.pop()


def nsa_consts(parity, S):
    bf = ml_dtypes.bfloat16
    r = np.arange(128)
    K = {}
    K["M0"] = (16.0 * r[:, None] - r[None, :]).astype(np.float32)
    K["par128"] = np.full((128, 1), 128.0 * parity, np.float32)
    tt_ = r[:, None]
    b = (tt_ >= 64).astype(np.int64)
    x = np.arange(256)[None, :]
    up = x - 2 * parity - 128
    keep = (up < b - 1).astype(np.float32)
    add = np.where(up == b, 1.0e4, np.where(up == b - 1, 2.0e4, np.where(up > b, NEG, 0.0))).astype(np.float32)
    K["keepW"] = keep
    K["addW"] = add
    tri_incl = (r[:, None] <= r[None, :]).astype(np.float32)
    supper = (r[:, None] > r[None, :]).astype(np.float32)
    ones = np.ones((128, 128), np.float32)
    zeros = np.zeros((128, 128), np.float32)
    if parity == 0:
        wm = [supper, ones, ones, ones, tri_incl, zeros]
    else:
        wm = [zeros, supper, ones, ones, ones, tri_incl]
    K["wmask"] = np.stack(wm, 1).astype(bf)
    nc_ = S // 16
    ncp = ((nc_ + 127) // 128) * 128
    c = np.arange(ncp)[:, None]
    n = np.arange(128)[None, :]
    ov = ((16 * c < 64 * n + 64) & (16 * c + 31 >= 64 * n) & (c < nc_ - 1)).astype(np.float32)
    K["overlap"] = ov.astype(bf)
    pos = (16 * np.arange(ncp) + 31).astype(np.float32)
    inv = (500000.0 ** (-np.arange(0, 16, 2, dtype=np.float32) / 16)).astype(np.float32)
    ang = pos[:, None] * inv[None, :]
    K["ropec"] = np.concatenate([np.cos(ang), np.sin(ang)], 1).astype(np.float32)
    return K


def dsa_pass(P, C, D_, K_, S, NTL, topk, niter=22):
    P.push()
    nblk = S // 128
    B = C.banks
    ident = C.ident
    DKT = P.sbuf("dDKT", [64, S], BF16)
    IKT = P.sbuf("dIKT", [32, S], BF16)
    dve_ = P.sbuf("dDVe", [128, nblk, 65], BF16)
    for c0 in range(0, S, 1024):
        P.dma("sp", DKT, DKT[:, c0:c0 + 1024], D_["DKT"], D_["DKT"][:, c0:c0 + 1024])
        P.dma("sp", IKT, IKT[:, c0:c0 + 1024], D_["IKT"], D_["IKT"][:, c0:c0 + 1024])
    mset(P, "pool", dve_, dve_[:], 1.0)
    for j0 in range(0, nblk, 8):
        P.dma("sp", dve_, dve_[:, j0:j0 + 8, 0:64], D_["DV"],
              D_["DV"][j0 * 128:(j0 + 8) * 128, :].rearrange("(j p) d -> p j d", p=128))
    dq = [P.sbuf("ddq%d" % i, [64, 4, 128], BF16) for i in range(2)]
    iq = [P.sbuf("diq%d" % i, [32, 8, 128], BF16) for i in range(2)]
    sg = [P.sbuf("dsg%d" % i, [128, 8], F32) for i in range(2)]
    dsg = [P.sbuf("ddsg%d" % i, [128, 8, 128], BF16) for i in range(2)]
    rl = [P.sbuf("drl%d" % i, [128, 512], BF16) for i in range(3)]
    score = P.sbuf("dscore", [128, S], F32)
    mask = P.sbuf("dmask", [128, S], BF16)
    bs = P.sbuf("dbs", [128, 8], F32)
    Eb = [P.sbuf("dE%d" % i, [128, 512], BF16) for i in range(2)]
    Pm = [P.sbuf("dPm%d" % i, [128, 512], BF16) for i in range(2)]
    dn = P.sbuf("ddn", [128, 8], F32)
    oc = [P.sbuf("doc%d" % i, [128, 4, 64], BF16) for i in range(2)]
    nl = 0
    for m in range(NTL // 128):
        J = 2 * m + 2
        nchk = (J + 3) // 4
        Sp = min(nchk * 512, S)
        dq_, iq_, sg_, dsg_ = dq[m % 2], iq[m % 2], sg[m % 2], dsg[m % 2]
        P.dma("sp", dq_, dq_[:], D_["DQT"], D_["DQT"][:, 0:4, m * 128:(m + 1) * 128])
        P.dma("sp", iq_, iq_[:], D_["IQT"], D_["IQT"][:, :, m * 128:(m + 1) * 128])
        P.dma("sp", sg_, sg_[:], D_["SGN"], D_["SGN"][m * 128:(m + 1) * 128, :])
        for h in range(8):
            ts(P, "pool" if h % 2 else "dve", dsg_, dsg_[:, h, :], ident, ident[:], sg_[:, h:h + 1], None, ALU.mult,
               extra=(sg_,))
        for ck in range(nchk):
            c0 = ck * 512
            cw = min(512, Sp - c0)
            scp = B[3 + ck % 2]
            for h in range(8):
                lg = B[nl % 3]
                r_ = rl[nl % 3]
                nl += 1
                mm(P, lg, lg[:, 0:cw], iq_, iq_[:, h, :], IKT, IKT[:, c0:c0 + cw], True, True)
                act(P, r_, r_[:, 0:cw], lg, lg[:, 0:cw], AF.Relu)
                mm(P, scp, scp[:, 0:cw], dsg_, dsg_[:, h, :], r_, r_[:, 0:cw], h == 0, h == 7)
            if ck == nchk - 1:
                which = 0 if (J % 4 == 2 and cw == 512) else 1
                tt(P, "dve", score, score[:, c0:c0 + cw], scp, scp[:, 0:cw], K_["imask"],
                   K_["imask"][:, which, 512 - cw:512], ALU.add)
            else:
                cp(P, "act" if ck % 2 else "dve", score, score[:, c0:c0 + cw], scp, scp[:, 0:cw])
        P.op("dve", lambda e, Sp=Sp: e.reduce_max(bs[:, 0:1], score[:, 0:Sp], AX.X), reads=(score,), writes=(bs,))
        ts(P, "dve", bs, bs[:, 1:2], bs, bs[:, 0:1], -128.0, None, ALU.add)
        for k in range(niter):
            w = 128.0 / (2.0 ** (k + 1))
            ts(P, "dve", bs, bs[:, 2:3], bs, bs[:, 1:2], w, None, ALU.add)
            ts(P, "dve", mask, mask[:, 0:Sp], score, score[:, 0:Sp], bs[:, 2:3], None, ALU.is_ge, op1=ALU.add,
               extra=(bs,), accum=bs[:, 3:4], accum_b=bs)
            ts(P, "dve", bs, bs[:, 4:5], bs, bs[:, 3:4], topk - 0.5, w, ALU.is_ge, ALU.mult)
            tt(P, "dve", bs, bs[:, 1:2], bs, bs[:, 1:2], bs, bs[:, 4:5], ALU.add)
        ts(P, "dve", mask, mask[:, 0:Sp], score, score[:, 0:Sp], bs[:, 1:2], None, ALU.is_ge, extra=(bs,))
        acc = B[7]
        accv = acc[:, 0:260].rearrange("p (h c) -> p h c", c=65)
        qv = dq_[:].rearrange("p h t -> p (h t)")
        for j in range(J):
            ks = slice(j * 128, (j + 1) * 128)
            mp = B[5]
            sp = B[6]
            mm(P, mp, mp[:, (j % 2) * 128:(j % 2) * 128 + 128], mask, mask[:, ks], ident, ident[:], True, True)
            mm(P, sp, sp[:, :], DKT, DKT[:, ks], dq_, qv, True, True)
            e_, p_ = Eb[j % 2], Pm[j % 2]
            act(P, e_, e_[:], sp, sp[:, :], AF.Exp)
            tt(P, "dve", p_, hv(p_[:], 128), e_, hv(e_[:], 128), mp,
               mp[:, (j % 2) * 128:(j % 2) * 128 + 128].unsqueeze(1).broadcast_to([128, 4, 128]), ALU.mult)
            for h in range(4):
                mm(P, acc, acc[:, h * 65:(h + 1) * 65], p_, p_[:, h * 128:(h + 1) * 128], dve_, dve_[:, j, :],
                   j == 0 and h == 0, j == J - 1 and h == 3)
        ts(P, "dve", dn, dn[:, 0:4], acc, accv[:, :, 64], 1e-30, None, ALU.max)
        P.op("dve", lambda e: e.reciprocal(dn[:, 4:8], dn[:, 0:4]), reads=(dn,), writes=(dn,))
        o_ = oc[m % 2]
        tt(P, "dve", o_, o_[:], acc, accv[:, :, 0:64], dn, dn[:, 4:8].unsqueeze(2).broadcast_to([128, 4, 64]), ALU.mult)
        P.dma("sp", D_["OC"], D_["OC"][m * 128:(m + 1) * 128, :], o_, o_[:].rearrange("p h d -> p (h d)"), sem_buf=o_)
    P.pop()


def dsa_consts(parity):
    r = np.arange(128)
    triA = np.where(r[None, :] <= r[:, None], 0.0, NEG).astype(np.float32)
    Z = np.zeros((128, 128), np.float32)
    N = np.full((128, 128), NEG, np.float32)
    c0, c1 = (triA, N) if parity == 0 else (Z, triA)
    mA = np.concatenate([c0, c1, N, N], 1)
    mB = np.concatenate([Z, Z, c0, c1], 1)
    return {"imask": np.stack([mA, mB], 1).astype(np.float32)}


def merge_pass(P, C, D_, NTL, G=4):
    P.push()
    GT_ = G * 128
    B = C.banks
    ident = C.ident
    wbr = P.sbuf("mwbr", [128, 8, D], BF16)
    wo = P.sbuf("mwo", [128, 8, D], BF16)
    load_w(P, wbr, D_["wa"], 0, 256, 0, D)
    for kc in range(4):
        P.dma("pool", wbr, wbr[:, 2 + kc, :], D_["wb"], D_["wb"][kc * 128:(kc + 1) * 128, :])
    for kc in range(2):
        P.dma("pool", wbr, wbr[:, 6 + kc, :], D_["wc"], D_["wc"][kc * 128:(kc + 1) * 128, :])
    load_w(P, wo, D_["wo"], 0, D, 0, D)
    otm = [P.sbuf("motm%d" % t, [128, D], BF16) for t in range(G)]
    oT = P.sbuf("moT", [128, 8, GT_], BF16)
    gt = [P.sbuf("mgt%d" % i, [128, 3, GT_], BF16) for i in range(2)]
    t1 = [P.sbuf("mt1%d" % i, [128, GT_], F32) for i in range(2)]
    t2 = [P.sbuf("mt2%d" % i, [128, GT_], F32) for i in range(2)]
    mT = P.sbuf("mmT", [128, 8, GT_], BF16)
    xt = [P.sbuf("mxt%d" % t, [128, D], F32) for t in range(G)]
    branches = [(0, 0, 2), (1, 2, 4), (2, 6, 2)]
    for g in range(NTL // GT_):
        tok = slice(g * GT_, (g + 1) * GT_)
        for t in range(G):
            rows = slice(g * GT_ + t * 128, g * GT_ + (t + 1) * 128)
            P.dma("sp", otm[t], otm[t][:, 0:256], D_["OA"], D_["OA"][rows, :])
            P.dma("sp", otm[t], otm[t][:, 256:768], D_["OB"], D_["OB"][rows, :])
            P.dma("sp", otm[t], otm[t][:, 768:1024], D_["OC"], D_["OC"][rows, :])
            P.dma("sp", xt[t], xt[t][:], D_["x1"], D_["x1"][rows, :])
        for kc in range(8):
            pb = B[kc % 2]
            pv = bank_bf(pb)
            for t in range(G):
                tr(P, pb, pv[:, t * 128:(t + 1) * 128], otm[t], otm[t][:, kc * 128:(kc + 1) * 128], ident, ident[:])
            cp(P, "act" if kc % 2 else "dve", oT, oT[:, kc, :], pb, pv[:, 0:GT_])
        for f in range(8):
            g_ = gt[f % 2]
            for b in range(3):
                P.dma("sp", g_, g_[:, b, :], D_["GT"], D_["GT"][8 * b + f, :, tok])
            a1, a2 = t1[f % 2], t2[f % 2]
            for (b, k0, nk) in branches:
                yp = B[2 + b]
                for kc in range(nk):
                    mm(P, yp, yp[:, 0:GT_], wbr, wbr[:, k0 + kc, f * 128:(f + 1) * 128], oT, oT[:, k0 + kc, :],
                       kc == 0, kc == nk - 1)
                if b == 0:
                    tt(P, "dve", a1, a1[:], yp, yp[:, 0:GT_], g_, g_[:, 0, :], ALU.mult)
                else:
                    tt(P, "dve", a2, a2[:], yp, yp[:, 0:GT_], g_, g_[:, b, :], ALU.mult)
                    if b == 1:
                        tt(P, "pool", a1, a1[:], a1, a1[:], a2, a2[:], ALU.add)
                    else:
                        tt(P, "pool", mT, mT[:, f, :], a1, a1[:], a2, a2[:], ALU.add)
        for t in range(G):
            for hf in range(2):
                yp = B[6 + hf]
                for f in range(8):
                    mm(P, yp, yp[:, :], mT, mT[:, f, t * 128:(t + 1) * 128], wo, wo[:, f, hf * 512:(hf + 1) * 512],
                       f == 0, f == 7)
                tt(P, "dve", xt[t], xt[t][:, hf * 512:(hf + 1) * 512], yp, yp[:, :], xt[t],
                   xt[t][:, hf * 512:(hf + 1) * 512], ALU.add)
            rows = slice(g * GT_ + t * 128, g * GT_ + (t + 1) * 128)
            P.dma("sp", D_["x2"], D_["x2"][rows, :], xt[t], xt[t][:], sem_buf=xt[t])
    P.pop()


S_FULL = 8192
NT_OWN = 4096
BF = ml_dtypes.bfloat16


def _np_dt(dt):
    return BF if dt == BF16 else np.float32


def _consts_for(parity, S):
    K = host_consts(parity)
    K.update(nsa_consts(parity, S))
    K.update(dsa_consts(parity))
    return K


def _load_consts(P, names, S):
    proto = _consts_for(0, S)
    cn = {}
    for k in names:
        v = proto[k]
        cn[k] = P.dram("c_" + k, list(v.shape), BF16 if v.dtype == BF else F32, "ExternalInput")
    return cn, proto


def _sb_const(P, cn, proto, k):
    v = proto[k]
    b = P.sbuf("k_" + k, list(v.shape), BF16 if v.dtype == BF else F32)
    P.dma("sp", b, b[:], cn[k], cn[k][:])
    return b


def _ffn_inputs(P, sfx):
    return (P.dram("fg" + sfx, [D], F32, "ExternalInput"),
            P.dram("fwgu" + sfx, [D, 2 * DFF], F32, "ExternalInput"),
            P.dram("fwd" + sfx, [DFF, D], F32, "ExternalInput"))


def _proj_inputs(P, NT):
    return dict(g=P.dram("pg", [D], F32, "ExternalInput"), win=P.dram("pwin", [D, DIN], F32, "ExternalInput"),
                wgate=P.dram("pwgate", [D, 3 * D], F32, "ExternalInput"),
                gains=P.dram("pgains", [5, 64], F32, "ExternalInput"), rope=P.dram("prope", [NT, 24], F32, "ExternalInput"))


def build_A0(NT):
    P = Prog()
    x = P.dram("x", [NT, D], F32, "ExternalInput")
    f1 = _ffn_inputs(P, "1")
    pi = _proj_inputs(P, NT)
    cn, proto = _load_consts(P, ["ident"], 1024)
    x1 = P.dram("x1", [NT, D], F32, "ExternalOutput")
    O = {k: P.dram("o_" + k, f(NT), dt, "ExternalOutput") for k, (f, dt) in PROJ_OUT.items()}
    C = make_ctx(P, cn["ident"])
    P.push(); ffn_pass(P, C, x, x1, f1[0], f1[1], f1[2], NT, 4, "f1"); P.pop()
    P.push(); proj_pass(P, C, x1, pi["g"], pi["win"], pi["wgate"], pi["gains"], pi["rope"], O, NT, 4, "p1"); P.pop()
    return P.finish()


def build_SB(S, NT):
    P = Prog()
    QT = P.dram("QT_sb", [64, 4, NT], BF16, "ExternalInput")
    KT = P.dram("KT_sb", [64, 4, S], BF16, "ExternalInput")
    V = P.dram("V_sb", [S, 256], BF16, "ExternalInput")
    cn, proto = _load_consts(P, ["ident", "uincl", "ones", "sbm"], S)
    OA = P.dram("OA", [NT, 256], BF16, "ExternalOutput")
    C = make_ctx(P, cn["ident"])
    K_ = {k: _sb_const(P, cn, proto, k) for k in ("uincl", "ones", "sbm")}
    sb_pass(P, C, QT, KT, V, OA, K_, S, NT)
    return P.finish()


def build_NSA(S, NT):
    P = Prog()
    shp = {"NQT": [64, 8, NT], "NKT": [64, 4, S], "NVS": [S, 2, 64], "NVW": [S, 2, 64], "KCT": [64, 2, S],
           "VCT": [64, 2, S]}
    D_ = {k: P.dram(k, v, BF16, "ExternalInput") for k, v in shp.items()}
    NCP = ((S // 16 + 127) // 128) * 128
    for k, v in {"NG": [NT, 24], "pe": [2, 32, 64], "w1": [2, 2048, 128], "w2": [2, 128, 64], "kn0": [1, 64],
                 "ropec": [NCP, 16]}.items():
        D_[k] = P.dram(k, v, F32, "ExternalInput")
    D_["OB"] = P.dram("OB", [NT, 512], BF16, "ExternalOutput")
    names = ["ident", "cim", "M0", "par128", "keepW", "addW", "wmask", "overlap"]
    cn, proto = _load_consts(P, names, S)
    C = make_ctx(P, cn["ident"])
    K_ = {"overlap": cn["overlap"]}
    for k in names[1:-1]:
        K_[k] = _sb_const(P, cn, proto, k)
    nsa_pass(P, C, D_, K_, S, NT)
    return P.finish()


def build_DSA(S, NT):
    P = Prog()
    shp = {"DQT": [64, 5, NT], "DKT": [64, S], "DV": [S, 64], "IQT": [32, 8, NT], "IKT": [32, S]}
    D_ = {k: P.dram(k, v, BF16, "ExternalInput") for k, v in shp.items()}
    D_["SGN"] = P.dram("SGN", [NT, 8], F32, "ExternalInput")
    D_["OC"] = P.dram("OC", [NT, 256], BF16, "ExternalOutput")
    cn, proto = _load_consts(P, ["ident", "imask"], S)
    C = make_ctx(P, cn["ident"])
    K_ = {"imask": _sb_const(P, cn, proto, "imask")}
    dsa_pass(P, C, D_, K_, S, NT, min(256, S // 4))
    return P.finish()


def build_M(NT, last):
    P = Prog()
    D_ = {}
    for k, v in {"OA": [NT, 256], "OB": [NT, 512], "OC": [NT, 256], "GT": [24, 128, NT]}.items():
        D_[k] = P.dram(k, v, BF16, "ExternalInput")
    for k, v in {"x1": [NT, D], "wa": [256, D], "wb": [512, D], "wc": [256, D], "wo": [D, D]}.items():
        D_[k] = P.dram(k, v, F32, "ExternalInput")
    f2 = _ffn_inputs(P, "2")
    cn, proto = _load_consts(P, ["ident"], 1024)
    if last:
        xo = P.dram("xout", [NT, D], F32, "ExternalOutput")
    else:
        f1 = _ffn_inputs(P, "1")
        pi = _proj_inputs(P, NT)
        xo = P.dram("x3", [NT, D], F32, "Internal")
        x1n = P.dram("x1n", [NT, D], F32, "ExternalOutput")
        O = {k: P.dram("o_" + k, f(NT), dt, "ExternalOutput") for k, (f, dt) in PROJ_OUT.items()}
    D_["x2"] = P.dram("x2", [NT, D], F32, "Internal")
    C = make_ctx(P, cn["ident"])
    merge_pass(P, C, D_, NT)
    P.push(); ffn_pass(P, C, D_["x2"], xo, f2[0], f2[1], f2[2], NT, 4, "f2"); P.pop()
    if not last:
        P.push(); ffn_pass(P, C, xo, x1n, f1[0], f1[1], f1[2], NT, 4, "f1"); P.pop()
        P.push(); proj_pass(P, C, x1n, pi["g"], pi["win"], pi["wgate"], pi["gains"], pi["rope"], O, NT, 4, "p1"); P.pop()
    return P.finish()


def _rope_own(parity, S, NT):
    own = (np.arange(NT // 128)[:, None] * 2 + parity) * 128 + np.arange(128)[None, :]
    pos = own.reshape(-1).astype(np.float32)

    def tab(rot):
        inv = (500000.0 ** (-np.arange(0, rot, 2, dtype=np.float32) / rot)).astype(np.float32)
        ang = pos[:, None] * inv[None, :]
        return np.cos(ang), np.sin(ang)

    c, s = tab(16)
    ci, si = tab(8)
    return np.concatenate([c, s, ci, si], 1).astype(np.float32)


def _interleave(a0, a1, axis):
    a0 = np.moveaxis(np.asarray(a0), axis, 0)
    a1 = np.moveaxis(np.asarray(a1), axis, 0)
    n = a0.shape[0] // 128
    st = np.stack([a0.reshape((n, 128) + a0.shape[1:]), a1.reshape((n, 128) + a1.shape[1:])], 1)
    full = st.reshape((2 * n * 128,) + a0.shape[1:])
    return np.ascontiguousarray(np.moveaxis(full, 0, axis))


_PROGS = {}


def _prog(key, fn):
    if key not in _PROGS:
        _PROGS[key] = fn()
    return _PROGS[key]


def kernel(**inp):
    S, NT, NCORE = S_FULL, NT_OWN, 8
    cores = list(range(NCORE))
    x = np.asarray(inp["x"], np.float32)
    Bn = x.shape[0]
    consts = [_consts_for(c % 2, S) for c in cores]
    ropes = [_rope_own(c % 2, S, NT) for c in cores]

    def own_rows(a, c):
        t = a.reshape((S // 256, 2, 128) + a.shape[1:])[:, c % 2]
        return np.ascontiguousarray(t.reshape((NT,) + a.shape[1:]))

    def cmap(c, names):
        return {"c_" + k: consts[c][k] for k in names}

    def ffn_w(l, which, sfx):
        return {"fg" + sfx: np.asarray(inp["ffn%d_norm" % which][l]), "fwgu" + sfx: np.asarray(inp["ffn%d_w_gu" % which][l]),
                "fwd" + sfx: np.asarray(inp["ffn%d_w_down" % which][l])}

    def proj_w(l, c):
        gains = np.stack([inp["nsa_q_norm"][l], inp["nsa_k_norm"][l][1], inp["nsa_k_norm"][l][2],
                          inp["dsa_q_norm"][l], inp["dsa_k_norm"][l]]).astype(np.float32)
        return {"pg": np.asarray(inp["mix_norm"][l]), "pwin": np.asarray(inp["w_in"][l]),
                "pwgate": np.asarray(inp["w_gate"][l]), "pgains": gains, "prope": ropes[c]}

    def run(nc, maps):
        return run_bass_kernel_spmd(nc, maps, core_ids=cores).results

    ncA = _prog("A0", lambda: build_A0(NT))
    maps = []
    for c in cores:
        m = {"x": own_rows(x[c // 2], c)}
        m.update(ffn_w(0, 1, "1")); m.update(proj_w(0, c)); m.update(cmap(c, ["ident"]))
        maps.append(m)
    R = run(ncA, maps)
    x1 = [R[c]["x1"] for c in cores]
    for l in range(2):
        PO = [{k: R[c]["o_" + k] for k in PROJ_OUT} for c in cores]

        def full(name, axis, c):
            b = c // 2
            return _interleave(PO[2 * b][name], PO[2 * b + 1][name], axis)

        nc_ = _prog("SB", lambda: build_SB(S, NT))
        maps = []
        for c in cores:
            m = {"QT_sb": PO[c]["QT_sb"], "KT_sb": full("KT_sb", 2, c), "V_sb": full("V_sb", 0, c)}
            m.update(cmap(c, ["ident", "uincl", "ones", "sbm"]))
            maps.append(m)
        Ra = run(nc_, maps)
        nc_ = _prog("NSA", lambda: build_NSA(S, NT))
        maps = []
        for c in cores:
            m = {"NQT": PO[c]["NQT"], "NKT": full("NKT", 2, c), "NVS": full("NVS", 0, c), "NVW": full("NVW", 0, c),
                 "KCT": full("KCT", 2, c), "VCT": full("VCT", 2, c), "NG": PO[c]["NG"],
                 "pe": np.asarray(inp["nsa_cmp_pe"][l]), "w1": np.asarray(inp["nsa_cmp_w1"][l]),
                 "w2": np.asarray(inp["nsa_cmp_w2"][l]), "kn0": np.asarray(inp["nsa_k_norm"][l][0:1]),
                 "ropec": consts[c]["ropec"]}
            m.update(cmap(c, ["ident", "cim", "M0", "par128", "keepW", "addW", "wmask", "overlap"]))
            maps.append(m)
        Rb = run(nc_, maps)
        nc_ = _prog("DSA", lambda: build_DSA(S, NT))
        maps = []
        for c in cores:
            dkt = full("DQT", 2, c)[:, 4, :]
            m = {"DQT": PO[c]["DQT"], "DKT": np.ascontiguousarray(dkt), "DV": full("DV", 0, c), "IQT": PO[c]["IQT"],
                 "IKT": full("IKT", 1, c), "SGN": PO[c]["SGN"]}
            m.update(cmap(c, ["ident", "imask"]))
            maps.append(m)
        Rc = run(nc_, maps)
        last = (l == 1)
        nc_ = _prog("M%d" % int(last), lambda: build_M(NT, last))
        maps = []
        for c in cores:
            m = {"OA": Ra[c]["OA"], "OB": Rb[c]["OB"], "OC": Rc[c]["OC"], "GT": PO[c]["GT"], "x1": x1[c],
                 "wa": np.asarray(inp["w_br_a"][l]), "wb": np.asarray(inp["w_br_b"][l]),
                 "wc": np.asarray(inp["w_br_c"][l]), "wo": np.asarray(inp["w_out"][l])}
            m.update(ffn_w(l, 2, "2")); m.update(cmap(c, ["ident"]))
            if not last:
                m.update(ffn_w(l + 1, 1, "1")); m.update(proj_w(l + 1, c))
            maps.append(m)
        R = run(nc_, maps)
        if not last:
            x1 = [R[c]["x1n"] for c in cores]
    out = np.empty((Bn, S, D), np.float32)
    for c in cores:
        o = np.asarray(R[c]["xout"]).reshape(NT // 128, 128, D)
        out[c // 2].reshape(S // 256, 2, 128, D)[:, c % 2] = o
    return out
```
